# Optimizing a Trainium2 kernel written in Bass

```python
import jax, jax.numpy as jnp
from jax import lax
import numpy as np

D_MODEL = 1024
BATCH = 2
SEQ = 8192
DEPTH = 4

EPS = 1e-5
HEAD_DIM = 64
SSD_HEADS = 8
SSD_WIDTH = SSD_HEADS * HEAD_DIM
SSD_GROUPS = 2
SSD_STATE = 128
SSD_CHUNK = 128
CONV_K = 4
XBC_WIDTH = SSD_WIDTH + 2 * SSD_GROUPS * SSD_STATE
MOBA_HEADS = 4
MOBA_WIDTH = MOBA_HEADS * HEAD_DIM
MOBA_BLOCK = 256
MOBA_TOPK = 3
MOBA_QBLOCK = 64
SWA_HEADS = 4
SWA_KV_HEADS = 2
SWA_WIDTH = SWA_HEADS * HEAD_DIM
SWA_KV_WIDTH = SWA_KV_HEADS * HEAD_DIM
SWA_WINDOW = 128
SWA_BLOCK = 128
MIX_WIDTH = SSD_WIDTH + MOBA_WIDTH + SWA_WIDTH
N_IN = SSD_WIDTH + XBC_WIDTH + SSD_HEADS + 3 * MOBA_WIDTH + SWA_WIDTH + 2 * SWA_KV_WIDTH
PEER_HEADS = 8
PEER_TOPK = 16
PEER_NKEYS = 128
PEER_EXPERTS = PEER_NKEYS * PEER_NKEYS
PEER_DKEY = 256
PEER_HALF = PEER_DKEY // 2
PEER_CHUNK = 128

kernel_name = "hymba_ssd_moba_swa_peer_adaln"


def rms_norm(x, g):
    x32 = x.astype(jnp.float32)
    y = x32 * lax.rsqrt(jnp.mean(x32 * x32, axis=-1, keepdims=True) + EPS)
    return y.astype(x.dtype) * g.astype(x.dtype)


def causal_depthwise_conv(x, w, b):
    y = lax.conv_general_dilated(
        x, w.astype(x.dtype)[:, None, :], window_strides=(1,), padding=[(CONV_K - 1, 0)],
        dimension_numbers=("NWC", "WIO", "NWC"), feature_group_count=x.shape[-1])
    return y + b.astype(x.dtype)


def segsum(a):
    t = a.shape[-1]
    xr = jnp.broadcast_to(a[..., None], a.shape + (t,))
    xr = jnp.where(jnp.tril(jnp.ones((t, t), bool), -1), xr, 0.0)
    cs = jnp.cumsum(xr, axis=-2)
    return jnp.where(jnp.tril(jnp.ones((t, t), bool)), cs, -jnp.inf)


def ssd_mixer(z, xbc, dt_raw, conv_w, conv_b, dt_bias, a_log, d_skip, norm_g):
    f32 = jnp.float32
    b, s, _ = z.shape
    G, R, L, P, N = SSD_GROUPS, SSD_HEADS // SSD_GROUPS, SSD_CHUNK, HEAD_DIM, SSD_STATE
    nc = s // L
    xbc = jax.nn.silu(causal_depthwise_conv(xbc, conv_w, conv_b))
    xs, bm, cm = jnp.split(xbc, [SSD_WIDTH, SSD_WIDTH + G * N], axis=-1)
    dt = jax.nn.softplus(dt_raw.astype(f32) + dt_bias.astype(f32))
    a_head = -jnp.exp(a_log.astype(f32))
    xh = xs.reshape(b, nc, L, G, R, P)
    xdt = xh.astype(f32) * dt.reshape(b, nc, L, G, R)[..., None]
    bc = bm.reshape(b, nc, L, G, N)
    cc = cm.reshape(b, nc, L, G, N)
    a = (dt * a_head).reshape(b, nc, L, G, R).transpose(0, 3, 4, 1, 2)
    a_cs = jnp.cumsum(a, axis=-1)
    decay = jnp.exp(segsum(a))
    cb = jnp.einsum("bclgn,bcsgn->bgcls", cc, bc)
    y_diag = jnp.einsum("bgcls,bgrcls,bcsgrp->bclgrp", cb, decay, xdt)
    decay_states = jnp.exp(a_cs[..., -1:] - a_cs)
    states = jnp.einsum("bclgn,bgrcl,bclgrp->bcgrpn", bc, decay_states, xdt)
    states = jnp.concatenate([jnp.zeros_like(states[:, :1]), states], axis=1)
    last = jnp.pad(a_cs[..., -1], ((0, 0), (0, 0), (0, 0), (1, 0)))
    chunk_decay = jnp.exp(segsum(last))
    states = jnp.einsum("bgrzc,bcgrpn->bzgrpn", chunk_decay, states)[:, :-1]
    y_off = jnp.einsum("bclgn,bcgrpn,bgrcl->bclgrp", cc, states, jnp.exp(a_cs))
    y = (y_diag + y_off).reshape(b, s, SSD_HEADS, P) \
        + d_skip.astype(f32)[:, None] * xs.reshape(b, s, SSD_HEADS, P).astype(f32)
    y = y.reshape(b, s, SSD_WIDTH).astype(z.dtype)
    return rms_norm(y * jax.nn.silu(z), norm_g)


def moba_attention(q, k, v, norm_g):
    f32 = jnp.float32
    b, s, _ = q.shape
    H, Dh, BL, QB = MOBA_HEADS, HEAD_DIM, MOBA_BLOCK, MOBA_QBLOCK
    nb = -(-s // BL)
    pad = nb * BL - s
    q, k, v = (t.reshape(b, s, H, Dh).transpose(0, 2, 1, 3) for t in (q, k, v))
    k = jnp.pad(k, ((0, 0), (0, 0), (0, pad), (0, 0)))
    v = jnp.pad(v, ((0, 0), (0, 0), (0, pad), (0, 0)))
    kb = k.reshape(b, H, nb, BL, Dh)
    vb = v.reshape(b, H, nb, BL, Dh)
    k_mean = jnp.mean(kb.astype(f32), axis=3)
    gate = jnp.einsum("bhsd,bhnd->bhsn", q.astype(f32), k_mean)
    past = jnp.arange(nb)[None, :] < (jnp.arange(s) // BL)[:, None]
    gate = jnp.where(past, gate, -jnp.inf)
    topk = min(MOBA_TOPK, nb)
    _, sel = lax.top_k(gate, topk)
    nq = s // QB
    q_chunks = q.reshape(b, H, nq, QB, Dh).transpose(2, 0, 1, 3, 4)
    sel_chunks = sel.reshape(b, H, nq, QB, topk).transpose(2, 0, 1, 3, 4)
    bi = jnp.arange(b)[:, None, None, None]
    hi = jnp.arange(H)[None, :, None, None]
    scale = Dh ** -0.5

    def one_chunk(args):
        qc, selc, ci = args
        q0 = ci * QB
        blk = q0 // BL
        q_pos = q0 + jnp.arange(QB)
        k_sel = kb[bi, hi, selc]
        v_sel = vb[bi, hi, selc]
        s_sel = jnp.einsum("bhqd,bhqjkd->bhqjk", qc, k_sel).astype(f32) * scale
        valid = jnp.arange(topk) < blk
        s_sel = jnp.where(valid[:, None], s_sel, -jnp.inf).reshape(b, H, QB, topk * BL)
        start = blk * BL
        k_own = lax.dynamic_slice_in_dim(k, start, BL, axis=2)
        v_own = lax.dynamic_slice_in_dim(v, start, BL, axis=2)
        s_own = jnp.einsum("bhqd,bhkd->bhqk", qc, k_own).astype(f32) * scale
        k_pos = start + jnp.arange(BL)
        s_own = jnp.where(k_pos[None, :] <= q_pos[:, None], s_own, -jnp.inf)
        p = jax.nn.softmax(jnp.concatenate([s_sel, s_own], axis=-1), axis=-1).astype(v.dtype)
        p_sel = p[..., :topk * BL].reshape(b, H, QB, topk, BL)
        p_own = p[..., topk * BL:]
        return jnp.einsum("bhqjk,bhqjkd->bhqd", p_sel, v_sel) + jnp.einsum("bhqk,bhkd->bhqd", p_own, v_own)

    out = lax.map(one_chunk, (q_chunks, sel_chunks, jnp.arange(nq)))
    out = out.transpose(1, 0, 3, 2, 4).reshape(b, s, H * Dh)
    return rms_norm(out, norm_g)


def swa_attention(q, k, v, sinks, norm_g):
    f32 = jnp.float32
    b, s, _ = q.shape
    W, G, R, Dh = SWA_BLOCK, SWA_KV_HEADS, SWA_HEADS // SWA_KV_HEADS, HEAD_DIM
    nb = s // W
    qb = q.reshape(b, nb, W, G, R, Dh)
    kb = k.reshape(b, nb, W, G, Dh)
    vb = v.reshape(b, nb, W, G, Dh)
    shift = lambda t: jnp.concatenate([jnp.zeros_like(t[:, :1]), t[:, :-1]], axis=1)
    k_band = jnp.concatenate([shift(kb), kb], axis=2)
    v_band = jnp.concatenate([shift(vb), vb], axis=2)
    scores = jnp.einsum("bnqgrd,bnkgd->bngrqk", qb, k_band).astype(f32) * (Dh ** -0.5)
    i = jnp.arange(W)[:, None]
    j = jnp.arange(2 * W)[None, :]
    rel = i + W - j
    in_win = (rel >= 0) & (rel < SWA_WINDOW)
    has_prev = (jnp.arange(nb) > 0)[:, None, None] | (j >= W)[None]
    mask = in_win[None] & has_prev
    scores = jnp.where(mask[None, :, None, None], scores, -jnp.inf)
    sink = jnp.broadcast_to(sinks.astype(f32).reshape(G, R)[None, None, :, :, None, None],
                            scores.shape[:-1] + (1,))
    p = jax.nn.softmax(jnp.concatenate([scores, sink], axis=-1), axis=-1)[..., :-1].astype(v.dtype)
    out = jnp.einsum("bngrqk,bnkgd->bnqgrd", p, v_band).reshape(b, s, SWA_WIDTH)
    return rms_norm(out, norm_g)


def peer_ffn(h, wq, k1, k2, u, v):
    f32 = jnp.float32
    b, s, d = h.shape
    nch = (b * s) // PEER_CHUNK
    hc = h.reshape(nch, PEER_CHUNK, d)

    def one_chunk(xc):
        q = (xc @ wq).reshape(PEER_CHUNK, PEER_HEADS, 2, PEER_HALF).astype(f32)
        s1 = jnp.einsum("thd,nd->thn", q[:, :, 0], k1.astype(f32))
        s2 = jnp.einsum("thd,nd->thn", q[:, :, 1], k2.astype(f32))
        v1, i1 = lax.top_k(s1, PEER_TOPK)
        v2, i2 = lax.top_k(s2, PEER_TOPK)
        cand = (v1[..., :, None] + v2[..., None, :]).reshape(PEER_CHUNK, PEER_HEADS, PEER_TOPK * PEER_TOPK)
        sc, ci = lax.top_k(cand, PEER_TOPK)
        e = jnp.take_along_axis(i1, ci // PEER_TOPK, axis=-1) * PEER_NKEYS \
            + jnp.take_along_axis(i2, ci % PEER_TOPK, axis=-1)
        g = jax.nn.softmax(sc, axis=-1)
        u_sel = u[e]
        v_sel = v[e]
        pre = jnp.einsum("td,thkd->thk", xc, u_sel).astype(f32)
        act = (jax.nn.gelu(pre, approximate=False) * g).astype(v.dtype)
        return jnp.einsum("thk,thkd->td", act, v_sel)

    return lax.map(one_chunk, hc).reshape(b, s, d)


def setup_inputs(seed: int = 0) -> dict:
    key = jax.random.key(seed)
    ks = jax.random.split(key, 24)
    f32 = jnp.float32
    nrm = lambda k, shape, scale: jax.random.normal(k, shape, f32) * scale
    L, D = DEPTH, D_MODEL
    dt0 = jnp.exp(jax.random.uniform(ks[8], (L, SSD_HEADS), f32) * (np.log(0.1) - np.log(0.001)) + np.log(0.001))
    return {
        "x": nrm(ks[0], (BATCH, SEQ, D), 1.0),
        "c": nrm(ks[1], (BATCH, D), 1.0),
        "ada_w": nrm(ks[2], (L, D, 6 * D), 0.5 * D ** -0.5),
        "ada_b": nrm(ks[3], (L, 6 * D), 0.01),
        "norm1_g": 1.0 + nrm(ks[4], (L, D), 0.02),
        "norm2_g": 1.0 + nrm(ks[5], (L, D), 0.02),
        "w_in": nrm(ks[6], (L, D, N_IN), D ** -0.5),
        "conv_w": nrm(ks[7], (L, CONV_K, XBC_WIDTH), CONV_K ** -0.5),
        "conv_b": nrm(ks[9], (L, XBC_WIDTH), 0.01),
        "dt_bias": dt0 + jnp.log(-jnp.expm1(-dt0)),
        "a_log": jnp.log(jax.random.uniform(ks[10], (L, SSD_HEADS), f32, 1.0, 16.0)),
        "d_skip": 1.0 + nrm(ks[11], (L, SSD_HEADS), 0.1),
        "ssd_norm_g": 1.0 + nrm(ks[12], (L, SSD_WIDTH), 0.02),
        "moba_norm_g": 1.0 + nrm(ks[13], (L, MOBA_WIDTH), 0.02),
        "swa_sinks": nrm(ks[14], (L, SWA_HEADS), 0.5),
        "swa_norm_g": 1.0 + nrm(ks[15], (L, SWA_WIDTH), 0.02),
        "w_out": nrm(ks[16], (L, MIX_WIDTH, D), MIX_WIDTH ** -0.5),
        "peer_wq": nrm(ks[17], (L, D, PEER_HEADS * PEER_DKEY), D ** -0.5),
        "peer_k1": nrm(ks[18], (L, PEER_NKEYS, PEER_HALF), PEER_HALF ** -0.5),
        "peer_k2": nrm(ks[19], (L, PEER_NKEYS, PEER_HALF), PEER_HALF ** -0.5),
        "peer_u": nrm(ks[20], (L, PEER_EXPERTS, D), D ** -0.5),
        "peer_v": nrm(ks[21], (L, PEER_EXPERTS, D), PEER_HEADS ** -0.5),
        "final_g": 1.0 + nrm(ks[22], (D,), 0.02),
    }


def reference(x, c, ada_w, ada_b, norm1_g, norm2_g, w_in, conv_w, conv_b, dt_bias, a_log, d_skip,
              ssd_norm_g, moba_norm_g, swa_sinks, swa_norm_g, w_out, peer_wq, peer_k1, peer_k2,
              peer_u, peer_v, final_g):
    in_widths = [SSD_WIDTH, XBC_WIDTH, SSD_HEADS, MOBA_WIDTH, MOBA_WIDTH, MOBA_WIDTH,
                 SWA_WIDTH, SWA_KV_WIDTH, SWA_KV_WIDTH]
    offsets = np.cumsum(in_widths)[:-1].tolist()
    cond = jax.nn.silu(c)
    for l in range(DEPTH):
        mod = cond @ ada_w[l] + ada_b[l]
        sh1, sc1, g1, sh2, sc2, g2 = [m[:, None, :] for m in jnp.split(mod, 6, axis=-1)]
        h = rms_norm(x, norm1_g[l]) * (1.0 + sc1) + sh1
        proj = h @ w_in[l]
        z, xbc, dt_raw, mq, mk, mv, sq, sk, sv = jnp.split(proj, offsets, axis=-1)
        y_ssd = ssd_mixer(z, xbc, dt_raw, conv_w[l], conv_b[l], dt_bias[l], a_log[l], d_skip[l], ssd_norm_g[l])
        y_moba = moba_attention(mq, mk, mv, moba_norm_g[l])
        y_swa = swa_attention(sq, sk, sv, swa_sinks[l], swa_norm_g[l])
        y = jnp.concatenate([y_ssd, y_moba, y_swa], axis=-1) @ w_out[l]
        x = x + g1 * y
        h = rms_norm(x, norm2_g[l]) * (1.0 + sc2) + sh2
        x = x + g2 * peer_ffn(h, peer_wq[l], peer_k1[l], peer_k2[l], peer_u[l], peer_v[l])
    return rms_norm(x, final_g)
```

```python
import numpy as np
from contextlib import ExitStack
import concourse.bass as bass
import concourse.mybir as mybir
from concourse.bass_utils import run_bass_kernel_spmd

F32 = mybir.dt.float32
BF16 = mybir.dt.bfloat16
U32 = mybir.dt.uint32
ALU = mybir.AluOpType
AF = mybir.ActivationFunctionType
AX = mybir.AxisListType

D = 1024
NTM = 904
NFM = 1920
NBK = 32
BIGR = 240000.0
NEXP = 16384


class Prog:
    def __init__(self, nc, es):
        self.nc = nc
        self.es = es
        self.sem_es = es
        self.engs = {'pe': nc.tensor, 'dve': nc.vector, 'act': nc.scalar, 'pool': nc.gpsimd, 'sp': nc.sync}
        self.last = {k: None for k in self.engs}
        self.cur = {k: None for k in self.engs}
        self.cnt = {k: 0 for k in self.engs}
        self.seen = {k: {f: None for f in self.engs} for k in self.engs}
        self.n = 0
        self.nsem = 0

    def _newsem(self, e):
        self.nsem += 1
        self.cur[e] = self.sem_es.enter_context(self.nc.semaphore("s_%s_%d" % (e, self.nsem)))
        self.cnt[e] = 0

    def op(self, e, fn, inc=1, selfsync=True):
        eng = self.engs[e]
        for f in self.engs:
            if f == e and not selfsync:
                continue
            l = self.last[f]
            if l is not None and self.seen[e][f] != l:
                eng.wait_ge(l[0], l[1])
                self.seen[e][f] = l
        if self.cur[e] is None or self.cnt[e] + inc > 30000:
            self._newsem(e)
        ins = fn(eng)
        ins.then_inc(self.cur[e], inc)
        self.cnt[e] += inc
        self.last[e] = (self.cur[e], self.cnt[e])
        self.n += 1
        return ins

    def finish(self):
        eng = self.engs['pool']
        for f in self.engs:
            l = self.last[f]
            if l is not None:
                eng.wait_ge(l[0], l[1])

    def sb(self, name, shape, dt=F32):
        return self.es.enter_context(self.nc.sbuf_tensor(name, shape, dt))

    def ps(self, name, shape, dt=F32):
        return self.es.enter_context(self.nc.psum_tensor(name, shape, dt))

    def dma(self, out, in_, e='sp', **kw):
        return self.op(e, lambda g: g.dma_start(out=out, in_=in_, **kw), inc=16)

    def mm(self, out, lhsT, rhs, start=True, stop=True, sync=True):
        return self.op('pe', lambda g: g.matmul(out, lhsT=lhsT, rhs=rhs, start=start, stop=stop), selfsync=sync)

    def tr(self, out, in_, ident):
        return self.op('pe', lambda g: g.transpose(out=out, in_=in_, identity=ident))

    def tt(self, out, in0, in1, op, e='dve'):
        return self.op(e, lambda g: g.tensor_tensor(out=out, in0=in0, in1=in1, op=op))

    def ts(self, out, in0, s1, s2=None, op0=ALU.mult, op1=None, e='dve'):
        if op1 is None:
            return self.op(e, lambda g: g.tensor_scalar(out=out, in0=in0, scalar1=s1, scalar2=None, op0=op0))
        return self.op(e, lambda g: g.tensor_scalar(out=out, in0=in0, scalar1=s1, scalar2=s2, op0=op0, op1=op1))

    def stt(self, out, in0, scalar, in1, op0, op1, e='dve'):
        return self.op(e, lambda g: g.scalar_tensor_tensor(out=out, in0=in0, scalar=scalar, in1=in1, op0=op0, op1=op1))

    def act(self, out, in_, func, bias=None, scale=None, accum=None):
        kw = {}
        if bias is not None:
            kw['bias'] = bias
        if scale is not None:
            kw['scale'] = scale
        if accum is not None:
            kw['accum_out'] = accum
        return self.op('act', lambda g: g.activation(out=out, in_=in_, func=func, **kw))

    def copy(self, out, in_, e='dve'):
        return self.op(e, lambda g: g.tensor_copy(out=out, in_=in_))

    def red(self, out, in_, op=ALU.add, absval=False):
        if absval:
            return self.op('dve', lambda g: g.tensor_reduce(out=out, in_=in_, axis=AX.X, op=op, apply_absolute_value=True))
        return self.op('dve', lambda g: g.tensor_reduce(out=out, in_=in_, axis=AX.X, op=op))

    def memset(self, ap, v, e='dve'):
        return self.op(e, lambda g: g.memset(ap, v))

    def recip(self, out, in_):
        return self.op('dve', lambda g: g.reciprocal(out=out, in_=in_))


def bc(ap, axis, shape):
    return ap.unsqueeze(axis).broadcast_to(list(shape))


def build_layer(NPRE, NOWN, dbg=False, stop_at=99):
    NS = NPRE + NOWN
    nc = bass.Bass("TRN2", target_bir_lowering=False)

    def din(name, shape):
        return nc.dram_tensor(name, list(shape), F32, kind="ExternalInput").ap()

    x_all = din("x_all", [NS * 128, D])
    valid_d = din("valid", [128, NS])
    gb_d = din("gb", [128, NOWN * NBK])
    gv_d = din("gv", [128, NOWN * NBK])
    own_d = din("ownm", [128, NOWN * NBK])
    prevb_d = din("prevb", [128, NOWN])
    fflag_d = din("fflag", [128, 1])
    c_d = din("c", [1, D])
    ada_w = din("ada_w", [D, 6 * D])
    ada_b = din("ada_b", [1, 6 * D])
    n1g = din("norm1_g", [1, D])
    n2g = din("norm2_g", [1, D])
    w_in = din("w_in", [D, NTM + NFM])
    conv_w = din("conv_w", [4, D])
    conv_b = din("conv_b", [1, D])
    dt_bias = din("dt_bias", [1, 8])
    a_log = din("a_log", [1, 8])
    d_skip = din("d_skip", [1, 8])
    ssd_g = din("ssd_norm_g", [1, 512])
    moba_g = din("moba_norm_g", [1, 256])
    swa_g = din("swa_norm_g", [1, 256])
    sinks_d = din("swa_sinks", [1, 4])
    w_out = din("w_out", [D, D])
    wq_d = din("peer_wq", [D, 2048])
    k1_d = din("peer_k1", [128, 128])
    k2_d = din("peer_k2", [128, 128])
    pu = din("peer_u", [NEXP, D])
    pv = din("peer_v", [NEXP, D])
    fin_g = din("final_g", [1, D])
    ident_d = din("identc", [128, 128])
    tri_d = din("tric", [128, 128])
    negu_d = din("neguc", [128, 128])
    negl_d = din("neglc", [128, 128])
    oh_d = din("ohc", [33, NBK * 128])
    iota_d = din("iotac", [128, 16])
    hm_d = din("hmc", [128, 2])
    x_out = nc.dram_tensor("x_out", [NOWN * 128, D], F32, kind="ExternalOutput").ap()
    if dbg:
        dbg_out = nc.dram_tensor("dbg", [128, 4096], F32, kind="ExternalOutput").ap()

    with ExitStack() as es:
        P = Prog(nc, es)
        pA = P.ps("pA", [128, 512]); pB = P.ps("pB", [128, 512]); pC = P.ps("pC", [128, 512])
        pD = P.ps("pD", [128, 512]); pE = P.ps("pE", [128, 512]); pF = P.ps("pF", [128, 512])
        pG = P.ps("pG", [128, 512]); pT = P.ps("pT", [128, 1024], BF16)
        identf = P.sb("identf", [128, 128]); identb = P.sb("identb", [128, 128], BF16)
        tri = P.sb("tri", [128, 128]); negu = P.sb("negu", [128, 128]); negl = P.sb("negl", [128, 128])
        negub = P.sb("negub", [128, 128], BF16); neglb = P.sb("neglb", [128, 128], BF16)
        ohb = P.sb("ohb", [33, NBK, 128], BF16)
        iota16 = P.sb("iota16", [128, 16])
        ones1 = P.sb("ones1", [1, 128])
        valid = P.sb("validt", [128, NS]); gb = P.sb("gbt", [128, NOWN, NBK]); gv = P.sb("gvt", [128, NOWN, NBK])
        ownm = P.sb("ownt", [128, NOWN, NBK]); prevb = P.sb("prevbt", [128, NOWN]); fflag = P.sb("fflagt", [128, 1])
        P.dma(identf[:], ident_d[:, :]); P.dma(tri[:], tri_d[:, :]); P.dma(negu[:], negu_d[:, :]); P.dma(negl[:], negl_d[:, :])
        P.dma(iota16[:], iota_d[:, :])
        hm = P.sb("hm", [128, 2])
        P.dma(hm[:], hm_d[:, :])
        with ExitStack() as es2:
            ohf = es2.enter_context(nc.sbuf_tensor("ohf", [33, NBK * 128], F32))
            P.dma(ohf[:], oh_d[:, :])
            P.copy(ohb[:].rearrange("p a b -> p (a b)"), ohf[:])
        P.dma(valid[:], valid_d[:, :]); P.dma(gb[:].rearrange("p a b -> p (a b)"), gb_d[:, :])
        P.dma(gv[:].rearrange("p a b -> p (a b)"), gv_d[:, :]); P.dma(ownm[:].rearrange("p a b -> p (a b)"), own_d[:, :])
        P.dma(prevb[:], prevb_d[:, :]); P.dma(fflag[:], fflag_d[:, :])
        P.copy(identb[:], identf[:])
        P.copy(negub[:], negu[:]); P.copy(neglb[:], negl[:])
        P.memset(ones1[:], 1.0)

        def bload(name, src, n):
            t = P.sb(name, [128, n])
            P.dma(t[:], src[0:1, :].partition_broadcast(128))
            return t
        dtb = bload("dtb", dt_bias, 8); alog = bload("alog", a_log, 8); dskip = bload("dskip", d_skip, 8)
        ssdgb = bload("ssdgb", ssd_g, 512); mobagb = bload("mobagb", moba_g, 256); swagb = bload("swagb", swa_g, 256)
        sinkb = bload("sinkb", sinks_d, 4)
        Abc = P.sb("Abc", [128, 8])
        P.act(Abc[:], alog[:], AF.Exp)
        P.ts(Abc[:], Abc[:], -1.0)
        convw = P.sb("convw", [128, 8, 4]); convb = P.sb("convb", [128, 8])
        for k in range(4):
            P.dma(convw[:, :, k], conv_w[k:k + 1, :].rearrange("o (c p) -> p (o c)", p=128), allow_slow_non_contiguous=True)
        P.dma(convb[:], conv_b[0:1, :].rearrange("o (c p) -> p (o c)", p=128), allow_slow_non_contiguous=True)

        condT = P.sb("condT", [128, 8]); sgm = P.sb("sgm", [128, 8])
        P.dma(condT[:], c_d[0:1, :].rearrange("o (k p) -> p (o k)", p=128), allow_slow_non_contiguous=True)
        P.act(sgm[:], condT[:], AF.Sigmoid)
        P.tt(condT[:], condT[:], sgm[:], ALU.mult)
        def adaln(MODt, first, nrows, gidx, ngsrc):
            with ExitStack() as es2:
                wst = es2.enter_context(nc.sbuf_tensor("adast%d" % first, [128, 8, 512], F32))
                mrow = es2.enter_context(nc.sbuf_tensor("mrow%d" % first, [1, 512], F32))
                abrow = es2.enter_context(nc.sbuf_tensor("abrow%d" % first, [1, 512], F32))
                ngb = es2.enter_context(nc.sbuf_tensor("ngb%d" % first, [128, D], F32))
                P.dma(ngb[:], ngsrc[0:1, :].partition_broadcast(128))
                for cc in range(nrows * 2):
                    col = first * D + cc * 512
                    P.dma(wst[:], ada_w[:, col:col + 512].rearrange("(k p) n -> p k n", p=128))
                    P.dma(abrow[:], ada_b[0:1, col:col + 512])
                    for k in range(8):
                        P.mm(pA[0:1, :], condT[:, k:k + 1], wst[:, k, :], start=(k == 0), stop=(k == 7), sync=(k == 0))
                    P.tt(mrow[:], pA[0:1, :], abrow[:], ALU.add)
                    P.mm(pB[:, :], ones1[:, :], mrow[:, :])
                    P.copy(MODt[:].rearrange("p a d -> p (a d)")[:, cc * 512:(cc + 1) * 512], pB[:, :])
                P.stt(MODt[:, gidx, :], MODt[:, gidx, :], 1.0, ngb[:], ALU.add, ALU.mult)

        def rmsnorm_rstd(src, width, out_rstd, junk, ssq):
            P.act(junk, src, AF.Square, accum=ssq)
            P.ts(ssq, ssq, 1.0 / width, 1e-5, ALU.mult, ALU.add)
            P.act(ssq, ssq, AF.Sqrt)
            P.recip(out_rstd, ssq)

        esA = ExitStack()
        P.es = esA
        MOD = P.sb("MOD", [128, 2, D])
        adaln(MOD, 0, 2, 1, n1g)
        SH1 = MOD[:, 0, :]; G1 = MOD[:, 1, :]

        winb = P.sb("winb", [128, 8, NTM + NFM], BF16)
        with ExitStack() as es2:
            st = es2.enter_context(nc.sbuf_tensor("wstage", [128, 8, 512], F32))
            tot = NTM + NFM
            for c0 in range(0, tot, 512):
                w_ = min(512, tot - c0)
                P.dma(st[:, :, 0:w_], w_in[:, c0:c0 + w_].rearrange("(k p) n -> p k n", p=128))
                P.copy(winb[:, :, c0:c0 + w_], st[:, :, 0:w_])

        S = P.sb("Sstate", [128, 8, 64])
        P.memset(S[:], 0.0)
        XB = P.sb("XB", [128, 8, 131])
        P.memset(XB[:], 0.0)
        KT = P.sb("KTst", [128, 2, NS * 128], BF16)
        VST = P.sb("VST", [128, NS, 4, 65], BF16)
        P.memset(VST[:].rearrange("p s h d -> p (s h d)"), 1.0)
        KM = P.sb("KM", [128, 2, NBK])
        P.memset(KM[:], 0.0)
        KAM = P.sb("KAM", [128, 2]); P.memset(KAM[:], 0.0)
        SKT = P.sb("SKT", [128, 2, 128], BF16)
        P.memset(SKT[:], 0.0)
        SVT = P.sb("SVT", [128, 2, 2, 65], BF16)
        P.memset(SVT[:].rearrange("p a g d -> p (a g d)"), 1.0)
        SKAM = P.sb("SKAM", [128, 1]); P.memset(SKAM[:], 0.0)

        xt = P.sb("xt", [128, D]); sq = P.sb("sq", [128, D]); ss = P.sb("ss", [128, 1]); rv = P.sb("rv", [128, 1])
        hb = P.sb("hb", [128, D], BF16); hT = P.sb("hT", [128, 8, 128], BF16)
        tm = P.sb("tm", [128, NTM]); fm = P.sb("fm", [128, 15, 128])
        cacc = P.sb("cacc", [128, 8, 128]); ctmp = sq[:].rearrange("p (c t) -> p c t", t=128); XA = cacc
        xs_tm = P.sb("xs_tm", [128, 8, 64]); B_tm = P.sb("B_tm", [128, 256])
        dt = P.sb("dt", [128, 8]); av = P.sb("av", [128, 8]); acs = P.sb("acs", [128, 8]); nacs = P.sb("nacs", [128, 8])
        abc = P.sb("abc", [128, 8, 128]); Eacs = P.sb("Eacs", [128, 8, 128]); wv = P.sb("wv", [128, 8])
        xdt = P.sb("xdt", [128, 8, 64]); xdtw = sq[:, 0:512].rearrange("p (h d) -> p h d", d=64)
        CBT = P.sb("CBT", [128, 2, 128]); DT = abc
        mix = P.sb("mix", [128, D]); ytm = P.sb("ytm", [128, 512]); sz = sq[:, 512:1024]
        CsT = mix[:].rearrange("p (h l) -> p h l", l=128)

        st8 = P.sb("st8", [128, 8]); st2 = P.sb("st2", [128, 2])
        QTb = P.sb("QTb", [128, 4, 128], BF16); Qf4 = XA[:, 0:4, :]; absq = XA[:, 4:8, :]
        gm = P.sb("gm", [128, 4, NBK]); m8 = P.sb("m8", [128, 4, 8]); sel = P.sb("sel", [128, 4, NBK])
        R = P.sb("Rm", [128, 4, 33]); RT = P.sb("RT", [33, 4, 128], BF16)
        PT = P.sb("PTt", [128, 4, 128], BF16)
        mo = P.sb("mo", [128, 4, 64]); rden = P.sb("rden", [128, 4]); accO = P.sb("accO", [128, 4, 65])
        SQb = QTb
        R2 = P.sb("R2", [128, 4, 2]); RT2 = P.sb("RT2", [2, 4, 128], BF16); sden = P.sb("sden", [128, 4])

        dcol = [0]

        def dump(ap, n):
            if dbg:
                P.dma(dbg_out[0:ap.shape[0], dcol[0]:dcol[0] + n], ap)
                dcol[0] += n

        def front(s):
            own = s >= NPRE
            j = s - NPRE
            par = s % 2
            P.dma(xt[:], x_all[s * 128:(s + 1) * 128, :])
            rmsnorm_rstd(xt[:], D, rv[:], sq[:], ss[:])
            P.tt(rv[:], rv[:], valid[:, s:s + 1], ALU.mult)
            P.stt(sq[:], xt[:], rv[:, 0:1], G1, ALU.mult, ALU.mult)
            P.stt(hb[:], SH1, valid[:, s:s + 1], sq[:], ALU.mult, ALU.add)
            for k in range(8):
                P.tr(pT[:, k * 128:(k + 1) * 128], hb[:, k * 128:(k + 1) * 128], identb[:])
            P.copy(hT[:].rearrange("p k t -> p (k t)"), pT[:, :])
            for (c0, w_, pp) in ((0, 512, pA), (512, NTM - 512, pB)):
                for k in range(8):
                    P.mm(pp[:, 0:w_], hT[:, k, :], winb[:, k, c0:c0 + w_], start=(k == 0), stop=(k == 7), sync=(k == 0))
            P.copy(tm[:, 0:512], pA[:, :]); P.copy(tm[:, 512:NTM], pB[:, 0:NTM - 512])
            banks = [pC, pD, pE, pF]
            for ch in range(15):
                pp = banks[ch // 4]
                for k in range(8):
                    P.mm(pp[:, (ch % 4) * 128:(ch % 4 + 1) * 128], winb[:, k, NTM + ch * 128:NTM + (ch + 1) * 128], hT[:, k, :],
                         start=(k == 0), stop=(k == 7), sync=(k == 0))
            for bi in range(4):
                n_ = 4 if bi < 3 else 3
                P.copy(fm[:, bi * 4:bi * 4 + n_, :].rearrange("p c t -> p (c t)"), banks[bi][:, 0:n_ * 128])
            P.copy(XB[:, :, 3:131], fm[:, 0:8, :])
            P.tt(cacc[:], XB[:, :, 0:128], bc(convw[:, :, 0], 2, [128, 8, 128]), ALU.mult)
            for k in range(1, 4):
                P.tt(ctmp[:], XB[:, :, k:k + 128], bc(convw[:, :, k], 2, [128, 8, 128]), ALU.mult)
                P.tt(cacc[:], cacc[:], ctmp[:], ALU.add)
            P.tt(cacc[:], cacc[:], bc(convb[:], 2, [128, 8, 128]), ALU.add)
            P.act(XA[:], cacc[:], AF.Silu)
            P.copy(ctmp[:, :, 0:3], XB[:, :, 128:131])
            P.copy(XB[:, :, 0:3], ctmp[:, :, 0:3])
            for c in range(4):
                P.tr(pA[:, c * 128:(c + 1) * 128], XA[:, c, :], identf[:])
            P.copy(xs_tm[:].rearrange("p h d -> p (h d)"), pA[:, :])
            for g in range(2):
                P.tr(pB[:, g * 128:(g + 1) * 128], XA[:, 4 + g, :], identf[:])
            P.copy(B_tm[:], pB[:, 0:256])
            P.tt(dt[:], tm[:, 512:520], dtb[:], ALU.add)
            P.act(dt[:], dt[:], AF.Exp)
            P.act(dt[:], dt[:], AF.Ln, bias=1.0)
            P.ts(dt[:], dt[:], valid[:, s:s + 1])
            P.tt(av[:], dt[:], Abc[:], ALU.mult)
            P.mm(pA[:, 0:8], tri[:], av[:])
            P.copy(acs[:], pA[:, 0:8])
            P.ts(nacs[:], acs[:], -1.0)
            P.copy(abc[:], bc(av[:], 2, [128, 8, 128]))
            for h in range(8):
                pp = pC if h < 4 else pD
                P.mm(pp[:, (h % 4) * 128:(h % 4 + 1) * 128], abc[:, h, :], tri[:])
            P.act(Eacs[:, 0:4, :].rearrange("p h l -> p (h l)"), pC[:, :], AF.Exp)
            P.act(Eacs[:, 4:8, :].rearrange("p h l -> p (h l)"), pD[:, :], AF.Exp)
            P.tt(wv[:, 0:4], pC[:, :].rearrange("p (h l) -> p h l", l=128)[:, :, 127], acs[:, 0:4], ALU.subtract)
            P.tt(wv[:, 4:8], pD[:, :].rearrange("p (h l) -> p h l", l=128)[:, :, 127], acs[:, 4:8], ALU.subtract)
            P.act(wv[:], wv[:], AF.Exp)
            P.tt(xdt[:], xs_tm[:], bc(dt[:], 2, [128, 8, 64]), ALU.mult)
            if own:
                for g in range(2):
                    P.mm(pE[:, g * 128:(g + 1) * 128], XA[:, 4 + g, :], XA[:, 6 + g, :])
                P.copy(CBT[:].rearrange("p g l -> p (g l)"), pE[:, 0:256])
                for h in range(8):
                    pp = pF if h < 4 else pG
                    o = pp[:, (h % 4) * 128:(h % 4 + 1) * 128]
                    P.mm(o, abc[:, h, :], tri[:], start=True, stop=False)
                    P.mm(o, identf[:], negu[:], start=False, stop=True, sync=False)
                for h in range(8):
                    pp = pF if h < 4 else pG
                    P.act(DT[:, h, :], pp[:, (h % 4) * 128:(h % 4 + 1) * 128], AF.Exp, bias=nacs[:, h:h + 1])
                for g in range(2):
                    P.tt(DT[:, g * 4:(g + 1) * 4, :], DT[:, g * 4:(g + 1) * 4, :], bc(CBT[:, g, :], 1, [128, 4, 128]), ALU.mult)
                    P.tt(CsT[:, g * 4:(g + 1) * 4, :], Eacs[:, g * 4:(g + 1) * 4, :], bc(XA[:, 6 + g, :], 1, [128, 4, 128]), ALU.mult)
                for h in range(8):
                    o = pE[:, h * 64:(h + 1) * 64]
                    P.mm(o, DT[:, h, :], xdt[:, h, :], start=True, stop=False)
                    P.mm(o, CsT[:, h, :], S[:, h, :], start=False, stop=True, sync=False)
                P.tt(ytm[:].rearrange("p (h d) -> p h d", d=64), xs_tm[:], bc(dskip[:], 2, [128, 8, 64]), ALU.mult)
                P.tt(ytm[:], ytm[:], pE[:, :], ALU.add)
                P.act(sz[:], tm[:, 0:512], AF.Silu)
                P.tt(ytm[:], ytm[:], sz[:], ALU.mult)
                rmsnorm_rstd(ytm[:], 512, rv[:], sq[:, 0:512], ss[:])
                P.stt(mix[:, 0:512], ytm[:], rv[:, 0:1], ssdgb[:], ALU.mult, ALU.mult)
            P.tt(xdtw[:], xdt[:], bc(wv[:], 2, [128, 8, 64]), ALU.mult)
            for g in range(2):
                P.mm(pA[:, g * 256:(g + 1) * 256], B_tm[:, g * 128:(g + 1) * 128],
                     xdtw[:, g * 4:(g + 1) * 4, :].rearrange("p h d -> p (h d)"))
            P.tt(S[:], S[:], bc(Eacs[:, :, 127], 2, [128, 8, 64]), ALU.mult)
            P.tt(S[:].rearrange("p h d -> p (h d)"), S[:].rearrange("p h d -> p (h d)"), pA[:, :], ALU.add)
            P.copy(KT[:, :, s * 128:(s + 1) * 128], fm[:, 10:12, :])
            P.red(st2[:], fm[:, 10:12, :], ALU.add)
            P.tt(KM[:, :, s // 2], KM[:, :, s // 2], st2[:], ALU.add)
            P.red(st2[:], fm[:, 10:12, :], ALU.max, absval=True)
            P.tt(KAM[:], KAM[:], st2[:], ALU.max)
            P.copy(VST[:, s, :, 0:64], tm[:, 520:776].rearrange("p (h d) -> p h d", d=64))
            P.copy(SKT[:, par, :], fm[:, 14, :])
            P.red(st2[:, 0:1], fm[:, 14, :], ALU.max, absval=True)
            P.tt(SKAM[:], SKAM[:], st2[:, 0:1], ALU.max)
            P.copy(SVT[:, par, :, 0:64], tm[:, 776:904].rearrange("p (g d) -> p g d", d=64))
            if own and stop_at > 5:
                back(s, j, par, stop_at)

        def back(s, j, par, stop_at=99):
            nblk = s // 2
            for h in range(4):
                P.ts(Qf4[:, h, :], fm[:, 8 + h // 2, :], hm[:, (h % 2):(h % 2) + 1])
            P.copy(QTb[:], Qf4[:])
            P.act(absq[:], Qf4[:], AF.Abs)
            for h in range(4):
                c = h // 2
                P.mm(pA[:, h * NBK:(h + 1) * NBK], Qf4[:, h, :], KM[:, c, :])
                P.mm(pB[:, h:h + 1], absq[:, h, :], KAM[:, c:c + 1])
            if stop_at <= 5.1:
                return
            P.stt(gm[:], pA[:, 0:4 * NBK].rearrange("p (h n) -> p h n", n=NBK), 1.0 / 256, bc(gb[:, j, :], 1, [128, 4, NBK]),
                  ALU.mult, ALU.add)
            for h in range(4):
                P.op('dve', lambda g: g.max(out=m8[:, h, :], in_=gm[:, h, :]))
            if stop_at <= 5.2:
                return
            P.tt(sel[:], gm[:], bc(m8[:, :, 2], 2, [128, 4, NBK]), ALU.is_ge)
            P.tt(sel[:], sel[:], bc(gv[:, j, :], 1, [128, 4, NBK]), ALU.mult)
            P.tt(sel[:], sel[:], bc(ownm[:, j, :], 1, [128, 4, NBK]), ALU.max)
            P.ts(R[:, :, 0:NBK], sel[:], 1.0, BIGR, ALU.subtract, ALU.mult)
            P.ts(R[:, :, 32], pB[:, 0:4], -1.0)
            if stop_at <= 5.3:
                return
            for h in range(4):
                P.tr(pC[0:33, h * 128:(h + 1) * 128], R[:, h, :], identf[:])
            P.copy(RT[:].rearrange("p h q -> p (h q)"), pC[0:33, :])
            if stop_at <= 5.4:
                return
            for kt in range(s + 1):
                n = kt // 2
                last = (kt == s)
                for h in range(4):
                    c = h // 2
                    o = pD[:, h * 128:(h + 1) * 128]
                    P.mm(o, KT[:, c, kt * 128:(kt + 1) * 128], QTb[:, h, :],
                         start=True, stop=False, sync=(h == 0))
                    if stop_at > 5.42:
                        P.mm(o, ohb[0:33, n, :], RT[:, h, :], start=False, stop=(not last), sync=False)
                    if last:
                        P.mm(o, identb[:], negub[:], start=False, stop=True, sync=False)
                P.act(PT[:].rearrange("p h q -> p (h q)"), pD[:, :], AF.Exp, scale=0.125)
                for h in range(4):
                    if stop_at <= 5.43:
                        break
                    P.mm(pE[:, h * 128:h * 128 + 65], PT[:, h, :], VST[:, kt, h, :], start=True, stop=True, sync=(h == 0))
                if stop_at > 5.43:
                    pEv0 = pE[:, :].rearrange("p (h d) -> p h d", d=128)[:, :, 0:65]
                    if kt == 0:
                        P.copy(accO[:], pEv0)
                    else:
                        P.tt(accO[:], accO[:], pEv0, ALU.add)
            if stop_at <= 5.5:
                return
            pEv = accO
            P.recip(rden[:], pEv[:, :, 64])
            P.tt(mo[:], pEv[:, :, 0:64], bc(rden[:], 2, [128, 4, 64]), ALU.mult)
            rmsnorm_rstd(mo[:].rearrange("p h d -> p (h d)"), 256, rv[:], sq[:, 0:256], ss[:])
            P.stt(mix[:, 512:768], mo[:].rearrange("p h d -> p (h d)"), rv[:, 0:1], mobagb[:], ALU.mult, ALU.mult)
            if stop_at <= 6:
                return
            heads = [(0, 0, 0), (1, 1, 0), (2, 0, 1), (3, 1, 1)]
            for (hq, c, g) in heads:
                P.ts(Qf4[:, hq, :], fm[:, 12 + c, :], hm[:, g:g + 1])
            P.copy(SQb[:], Qf4[:])
            P.act(absq[:], Qf4[:], AF.Abs)
            for (hq, c, g) in heads:
                P.mm(pB[:, hq:hq + 1], absq[:, hq, :], SKAM[:, 0:1])
            P.ts(R2[:, :, 1], pB[:, 0:4], -1.0)
            P.tt(R2[:, :, 0], R2[:, :, 1], bc(prevb[:, j:j + 1], 1, [128, 4, 1])[:, :, 0], ALU.add)
            for hq in range(4):
                P.tr(pC[0:2, hq * 128:(hq + 1) * 128], R2[:, hq, :], identf[:])
            P.copy(RT2[:].rearrange("p h q -> p (h q)"), pC[0:2, :])
            for ti, (kpar, negm) in enumerate(((1 - par, neglb), (par, negub))):
                for (hq, c, g) in heads:
                    o = pD[:, hq * 128:(hq + 1) * 128]
                    P.mm(o, SKT[:, kpar, :], SQb[:, hq, :], start=True, stop=False, sync=(hq == 0))
                    P.mm(o, ohb[0:2, ti, :], RT2[:, hq, :], start=False, stop=False, sync=False)
                    P.mm(o, identb[:], negm[:], start=False, stop=True, sync=False)
                P.act(PT[:].rearrange("p h q -> p (h q)"), pD[:, :], AF.Exp, scale=0.125)
                for (hq, c, g) in heads:
                    P.mm(pE[:, hq * 128:hq * 128 + 65], PT[:, hq, :], SVT[:, kpar, g, :], start=True, stop=True, sync=(hq == 0))
                pEv0 = pE[:, :].rearrange("p (h d) -> p h d", d=128)[:, :, 0:65]
                if ti == 0:
                    P.copy(accO[:], pEv0)
                else:
                    P.tt(accO[:], accO[:], pEv0, ALU.add)
            P.stt(sden[:], pB[:, 0:4], -0.125, sinkb[:], ALU.mult, ALU.add)
            P.act(sden[:], sden[:], AF.Exp)
            P.tt(sden[:], sden[:], pEv[:, :, 64], ALU.add)
            P.recip(rden[:], sden[:])
            P.tt(mo[:], pEv[:, :, 0:64], bc(rden[:], 2, [128, 4, 64]), ALU.mult)
            rmsnorm_rstd(mo[:].rearrange("p h d -> p (h d)"), 256, rv[:], sq[:, 0:256], ss[:])
            P.stt(mix[:, 768:1024], mo[:].rearrange("p h d -> p (h d)"), rv[:, 0:1], swagb[:], ALU.mult, ALU.mult)
            dump(mix[:], 1024)
            P.dma(x_out[j * 128:(j + 1) * 128, :], mix[:])

        for s in range(NS):
            if stop_at <= 3:
                break
            if stop_at == 4 and s >= NPRE:
                break
            front(s)
        esA.close()

        esB = ExitStack()
        P.es = esB
        if stop_at > 8:
            MODB = P.sb("MODB", [128, 4, D])
            adaln(MODB, 2, 4, 2, n2g)
            GT1 = MODB[:, 0, :]; SH2 = MODB[:, 1, :]; G2 = MODB[:, 2, :]; GT2 = MODB[:, 3, :]
            fingb = P.sb("fingb", [128, D])
            P.dma(fingb[:], fin_g[0:1, :].partition_broadcast(128))
            woutb = P.sb("woutb", [128, 8, D], BF16); wqb = P.sb("wqb", [128, 8, 2048], BF16)
            with ExitStack() as es2:
                st = es2.enter_context(nc.sbuf_tensor("wstageB", [128, 8, 512], F32))
                for c0 in range(0, D, 512):
                    P.dma(st[:], w_out[:, c0:c0 + 512].rearrange("(k p) n -> p k n", p=128))
                    P.copy(woutb[:, :, c0:c0 + 512], st[:])
                for c0 in range(0, 2048, 512):
                    P.dma(st[:], wq_d[:, c0:c0 + 512].rearrange("(k p) n -> p k n", p=128))
                    P.copy(wqb[:, :, c0:c0 + 512], st[:])
            kT = P.sb("kT", [128, 2, 128]); kst = P.sb("kst", [128, 128])
            for i_, kd in enumerate((k1_d, k2_d)):
                P.dma(kst[:], kd[:, :])
                P.tr(pA[:, 0:128], kst[:], identf[:])
                P.copy(kT[:, i_, :], pA[:, 0:128])
            xt = P.sb("xtB", [128, D]); mixn = P.sb("mixn", [128, D]); mixb = P.sb("mixbB", [128, D], BF16)
            mixT = P.sb("mixTB", [128, 8, 128], BF16); x1 = P.sb("x1B", [128, D]); h2 = P.sb("h2", [128, D])
            junk = P.sb("junkB", [128, D]); ss = P.sb("ssB", [128, 1]); rv = P.sb("rvB", [128, 1])
            qT = P.sb("qT", [128, 16, 128]); s12 = P.sb("s12", [128, 16, 128]); wk = P.sb("wkB", [128, 256])
            v16 = P.sb("v16", [128, 16, 16]); i16 = P.sb("i16", [128, 16, 16], U32); i16f = P.sb("i16f", [128, 16, 16])
            cand = P.sb("cand", [128, 8, 256]); eq = cand[:].rearrange("p h (k a) -> p h k a", a=16)
            sc = P.sb("sc", [128, 8, 16]); ci = P.sb("ci", [128, 8, 16], U32)
            au = P.sb("au", [128, 8, 16], U32); bu = P.sb("bu", [128, 8, 16], U32)
            af = P.sb("af", [128, 8, 16]); bf = P.sb("bf", [128, 8, 16])
            i1s = P.sb("i1s", [128, 8, 16]); i2s = P.sb("i2s", [128, 8, 16])
            ef = P.sb("ef", [128, 8, 16]); eu = P.sb("eu", [128, 128], U32)
            gg = P.sb("gg", [128, 8, 16]); zz = P.sb("zz", [128, 8])
            Gb = P.sb("Gb", [128, 8, D]); pre = P.sb("pre", [128, 8]); actv = P.sb("actv", [128, 8]); yacc = P.sb("yacc", [128, D])
            banks = [pC, pD, pE, pF]
            for j in range(NOWN):
                s_ = NPRE + j
                P.dma(xt[:], x_all[s_ * 128:(s_ + 1) * 128, :])
                P.dma(mixn[:], x_out[j * 128:(j + 1) * 128, :])
                P.copy(mixb[:], mixn[:])
                for k in range(8):
                    P.tr(pT[:, k * 128:(k + 1) * 128], mixb[:, k * 128:(k + 1) * 128], identb[:])
                P.copy(mixT[:].rearrange("p k t -> p (k t)"), pT[:, :])
                for (c0, pp) in ((0, pA), (512, pB)):
                    for k in range(8):
                        P.mm(pp[:, :], mixT[:, k, :], woutb[:, k, c0:c0 + 512], start=(k == 0), stop=(k == 7), sync=(k == 0))
                for (c0, pp) in ((0, pA), (512, pB)):
                    P.tt(x1[:, c0:c0 + 512], pp[:, :], GT1[:, c0:c0 + 512], ALU.mult)
                P.tt(x1[:], x1[:], xt[:], ALU.add)
                dump(x1[:], 1024)
                rmsnorm_rstd(x1[:], D, rv[:], junk[:], ss[:])
                P.stt(h2[:], x1[:], rv[:, 0:1], G2, ALU.mult, ALU.mult)
                P.tt(h2[:], h2[:], SH2, ALU.add)
                P.copy(mixb[:], h2[:])
                for k in range(8):
                    P.tr(pT[:, k * 128:(k + 1) * 128], mixb[:, k * 128:(k + 1) * 128], identb[:])
                P.copy(mixT[:].rearrange("p k t -> p (k t)"), pT[:, :])
                for hh in range(16):
                    pp = banks[(hh // 4) % 4]
                    for k in range(8):
                        P.mm(pp[:, (hh % 4) * 128:(hh % 4 + 1) * 128], wqb[:, k, hh * 128:(hh + 1) * 128], mixT[:, k, :],
                             start=(k == 0), stop=(k == 7), sync=(k == 0))
                for b4 in range(4):
                    P.copy(qT[:, b4 * 4:(b4 + 1) * 4, :].rearrange("p c t -> p (c t)"), banks[b4][:, :])
                for hh in range(16):
                    pp = banks[(hh // 4) % 4]
                    P.mm(pp[:, (hh % 4) * 128:(hh % 4 + 1) * 128], qT[:, hh, :], kT[:, hh % 2, :])
                for b4 in range(4):
                    P.copy(s12[:, b4 * 4:(b4 + 1) * 4, :].rearrange("p c t -> p (c t)"), banks[b4][:, :])
                for hh in range(16):
                    P.op('dve', lambda g: g.max(out=v16[:, hh, 0:8], in_=s12[:, hh, :]))
                    P.op('dve', lambda g: g.max_index(out=i16[:, hh, 0:8], in_max=v16[:, hh, 0:8], in_values=s12[:, hh, :]))
                    P.op('dve', lambda g: g.match_replace(out=wk[:, 0:128], in_to_replace=v16[:, hh, 0:8], in_values=s12[:, hh, :], imm_value=-1e30))
                    P.op('dve', lambda g: g.max(out=v16[:, hh, 8:16], in_=wk[:, 0:128]))
                    P.op('dve', lambda g: g.max_index(out=i16[:, hh, 8:16], in_max=v16[:, hh, 8:16], in_values=wk[:, 0:128]))
                v16v = v16[:].rearrange("p (h two) k -> p h two k", two=2)
                P.tt(cand[:].rearrange("p h (a b) -> p h a b", b=16), bc(v16v[:, :, 0, :], 3, [128, 8, 16, 16]),
                     bc(v16v[:, :, 1, :], 2, [128, 8, 16, 16]), ALU.add)
                for h in range(8):
                    P.op('dve', lambda g: g.max(out=sc[:, h, 0:8], in_=cand[:, h, :]))
                    P.op('dve', lambda g: g.max_index(out=ci[:, h, 0:8], in_max=sc[:, h, 0:8], in_values=cand[:, h, :]))
                    P.op('dve', lambda g: g.match_replace(out=wk[:, :], in_to_replace=sc[:, h, 0:8], in_values=cand[:, h, :], imm_value=-1e30))
                    P.op('dve', lambda g: g.max(out=sc[:, h, 8:16], in_=wk[:, :]))
                    P.op('dve', lambda g: g.max_index(out=ci[:, h, 8:16], in_max=sc[:, h, 8:16], in_values=wk[:, :]))
                P.op('dve', lambda g: g.tensor_single_scalar(out=au[:], in_=ci[:], scalar=4, op=ALU.logical_shift_right))
                P.op('dve', lambda g: g.tensor_single_scalar(out=bu[:], in_=ci[:], scalar=15, op=ALU.bitwise_and))
                P.copy(af[:], au[:]); P.copy(bf[:], bu[:]); P.copy(i16f[:], i16[:])
                i16fv = i16f[:].rearrange("p (h two) k -> p h two k", two=2)
                iob = iota16[:].unsqueeze(1).unsqueeze(1).broadcast_to([128, 8, 16, 16])
                for (sel_f, half, dst) in ((af, 0, i1s), (bf, 1, i2s)):
                    P.tt(eq, bc(sel_f[:], 3, [128, 8, 16, 16]), iob, ALU.is_equal)
                    P.tt(eq, eq, bc(i16fv[:, :, half, :], 2, [128, 8, 16, 16]), ALU.mult)
                    P.red(dst[:], eq, ALU.add)
                P.stt(ef[:], i1s[:], 128.0, i2s[:], ALU.mult, ALU.add)
                P.copy(eu[:], ef[:].rearrange("p h k -> p (h k)"))
                P.tt(gg[:], sc[:], bc(sc[:, :, 0], 2, [128, 8, 16]), ALU.subtract)
                P.act(gg[:], gg[:], AF.Exp)
                P.red(zz[:], gg[:], ALU.add)
                P.recip(zz[:], zz[:])
                P.tt(gg[:], gg[:], bc(zz[:], 2, [128, 8, 16]), ALU.mult)
                P.memset(yacc[:], 0.0)
                for h in range(8):
                    for grp in range(2):
                        k0 = h * 16 + grp * 8
                        for kk in range(8):
                            P.op('pool', lambda g: g.indirect_dma_start(
                                out=Gb[:, kk, :], out_offset=None, in_=pu[:, :],
                                in_offset=bass.IndirectOffsetOnAxis(ap=eu[:, k0 + kk:k0 + kk + 1], axis=0)), inc=16)
                        P.tt(Gb[:], Gb[:], bc(h2[:], 1, [128, 8, D]), ALU.mult)
                        P.red(pre[:], Gb[:], ALU.add)
                        P.act(actv[:], pre[:], AF.Gelu)
                        P.tt(actv[:], actv[:], gg[:, h, grp * 8:(grp + 1) * 8], ALU.mult)
                        for kk in range(8):
                            P.op('pool', lambda g: g.indirect_dma_start(
                                out=Gb[:, kk, :], out_offset=None, in_=pv[:, :],
                                in_offset=bass.IndirectOffsetOnAxis(ap=eu[:, k0 + kk:k0 + kk + 1], axis=0)), inc=16)
                        P.tt(Gb[:], Gb[:], bc(actv[:], 2, [128, 8, D]), ALU.mult)
                        P.red(junk[:], Gb[:].rearrange("p k d -> p d k"), ALU.add)
                        P.tt(yacc[:], yacc[:], junk[:], ALU.add)
                P.tt(yacc[:], yacc[:], GT2, ALU.mult)
                P.tt(x1[:], x1[:], yacc[:], ALU.add)
                rmsnorm_rstd(x1[:], D, rv[:], junk[:], ss[:])
                P.stt(h2[:], x1[:], rv[:, 0:1], fingb[:], ALU.mult, ALU.mult)
                P.tt(h2[:], h2[:], x1[:], ALU.subtract)
                P.stt(h2[:], h2[:], fflag[:, 0:1], x1[:], ALU.mult, ALU.add)
                P.dma(x_out[j * 128:(j + 1) * 128, :], h2[:])
        esB.close()
        P.finish()
        print("ops", P.n, "sems", P.nsem)
    return nc


def _perm_w_in(w):
    z = w[:, 0:512]; xbc = w[:, 512:1536]; dt = w[:, 1536:1544]; mq = w[:, 1544:1800]; mk = w[:, 1800:2056]
    mv = w[:, 2056:2312]; sq = w[:, 2312:2568]; sk = w[:, 2568:2696]; sv = w[:, 2696:2824]
    sqp = np.concatenate([sq[:, 0:64], sq[:, 128:192], sq[:, 64:128], sq[:, 192:256]], axis=1)
    return np.ascontiguousarray(np.concatenate([z, dt, mv, sv, xbc, mq, mk, sqp, sk], axis=1))


def _consts():
    r = np.arange(128)[:, None]; c = np.arange(128)[None, :]
    d = {}
    d["identc"] = np.eye(128, dtype=np.float32)
    d["tric"] = (r <= c).astype(np.float32)
    d["neguc"] = np.where(r <= c, 0.0, -BIGR).astype(np.float32)
    d["neglc"] = np.where(r > c, 0.0, -BIGR).astype(np.float32)
    oh = np.zeros((33, NBK, 128), np.float32)
    for n in range(NBK):
        oh[n, n, :] = 1.0
    oh[32, :, :] = 1.0
    d["ohc"] = oh.reshape(33, NBK * 128)
    hm = np.zeros((128, 2), np.float32); hm[:64, 0] = 1.0; hm[64:, 1] = 1.0
    d["hmc"] = hm
    d["iotac"] = np.tile(np.arange(16, dtype=np.float32)[None, :], (128, 1))
    return d


def _masks(NPRE, NOWN, npad):
    NS = NPRE + NOWN
    valid = np.zeros((128, NS), np.float32); valid[:, npad:] = 1.0
    gb = np.full((128, NOWN, NBK), -1e9, np.float32); gv = np.zeros((128, NOWN, NBK), np.float32)
    own = np.zeros((128, NOWN, NBK), np.float32); prevb = np.zeros((128, NOWN), np.float32)
    for j in range(NOWN):
        s = NPRE + j; b = s // 2
        for n in range(npad // 2, b):
            gb[:, j, n] = 0.0; gv[:, j, n] = 1.0
        own[:, j, b] = 1.0
        if s - 1 < npad:
            prevb[:, j] = -BIGR
    return dict(valid=valid, gb=gb.reshape(128, -1), gv=gv.reshape(128, -1), ownm=own.reshape(128, -1), prevb=prevb)


def kernel(x, c, ada_w, ada_b, norm1_g, norm2_g, w_in, conv_w, conv_b, dt_bias, a_log, d_skip,
           ssd_norm_g, moba_norm_g, swa_sinks, swa_norm_g, w_out, peer_wq, peer_k1, peer_k2,
           peer_u, peer_v, final_g):
    f32 = np.float32
    x = np.asarray(x, f32); c = np.asarray(c, f32)
    B, S, _ = x.shape
    NCORE = 8
    QPB = NCORE // B
    NOWN = S // QPB // 128
    NPRE = (QPB - 1) * NOWN
    NS = NPRE + NOWN
    depth = ada_w.shape[0]
    nc = build_layer(NPRE, NOWN)
    cst = _consts()
    msk = [_masks(NPRE, NOWN, (QPB - 1 - q) * NOWN) for q in range(QPB)]
    xcur = x.copy()
    row = lambda a: np.ascontiguousarray(np.asarray(a, f32)[None, :])
    for l in range(depth):
        shared = dict(cst)
        shared["ada_w"] = np.ascontiguousarray(ada_w[l], f32); shared["ada_b"] = row(ada_b[l])
        shared["norm1_g"] = row(norm1_g[l]); shared["norm2_g"] = row(norm2_g[l])
        shared["w_in"] = _perm_w_in(np.asarray(w_in[l], f32))
        shared["conv_w"] = np.ascontiguousarray(conv_w[l], f32); shared["conv_b"] = row(conv_b[l])
        shared["dt_bias"] = row(dt_bias[l]); shared["a_log"] = row(a_log[l]); shared["d_skip"] = row(d_skip[l])
        shared["ssd_norm_g"] = row(ssd_norm_g[l]); shared["moba_norm_g"] = row(moba_norm_g[l])
        shared["swa_norm_g"] = row(swa_norm_g[l]); shared["swa_sinks"] = row(swa_sinks[l])
        shared["w_out"] = np.ascontiguousarray(w_out[l], f32); shared["peer_wq"] = np.ascontiguousarray(peer_wq[l], f32)
        shared["peer_k1"] = np.ascontiguousarray(peer_k1[l], f32); shared["peer_k2"] = np.ascontiguousarray(peer_k2[l], f32)
        shared["peer_u"] = np.ascontiguousarray(peer_u[l], f32); shared["peer_v"] = np.ascontiguousarray(peer_v[l], f32)
        shared["final_g"] = row(final_g)
        shared["fflag"] = np.full((128, 1), 1.0 if l == depth - 1 else 0.0, f32)
        in_maps = []
        for core in range(NCORE):
            b, q = divmod(core, QPB)
            n = (q + 1) * NOWN * 128
            xall = np.zeros((NS * 128, D), f32)
            xall[NS * 128 - n:] = xcur[b, :n]
            d = dict(shared)
            d.update(msk[q])
            d["x_all"] = xall
            d["c"] = np.ascontiguousarray(c[b:b + 1])
            in_maps.append(d)
        res = run_bass_kernel_spmd(nc, in_maps, core_ids=list(range(NCORE)))
        for core in range(NCORE):
            b, q = divmod(core, QPB)
            xcur[b, q * NOWN * 128:(q + 1) * NOWN * 128] = res.results[core]["x_out"]
    return xcur
```
